# Optimizing a Trainium2 kernel written in Bass

```python
import jax, jax.numpy as jnp
from jax import lax
import numpy as np

D_MODEL = 2048
BATCH = 4
SEQ = 2048
DEPTH = 1

SWA_HEAD_DIM = 128
SWA_HEADS = (D_MODEL // 2) // SWA_HEAD_DIM
SWA_WIDTH = SWA_HEADS * SWA_HEAD_DIM
DILATED_PATTERNS = ((128, 1), (512, 4), (2048, 16))
SWA_BLOCK = 128

GLA_HEADS = 4
GLA_VALUE_DIM = (D_MODEL // 2) // GLA_HEADS
GLA_KEY_DIM = GLA_VALUE_DIM // 2
GLA_KEY_WIDTH = GLA_HEADS * GLA_KEY_DIM
GLA_VALUE_WIDTH = GLA_HEADS * GLA_VALUE_DIM
GLA_GATE_RANK = 16
GLA_GATE_TEMP = 16.0
GLA_CHUNK = 64

MIX_WIDTH = SWA_WIDTH + GLA_VALUE_WIDTH
IN_SPLITS = (
    SWA_WIDTH,
    2 * SWA_WIDTH,
    3 * SWA_WIDTH,
    3 * SWA_WIDTH + GLA_KEY_WIDTH,
    3 * SWA_WIDTH + 2 * GLA_KEY_WIDTH,
    3 * SWA_WIDTH + 2 * GLA_KEY_WIDTH + GLA_VALUE_WIDTH,
    3 * SWA_WIDTH + 2 * GLA_KEY_WIDTH + 2 * GLA_VALUE_WIDTH,
)
IN_WIDTH = IN_SPLITS[-1] + GLA_GATE_RANK

N_EXPERTS = 256
TOP_K = 8
N_GROUPS = 8
TOPK_GROUPS = 4
EXPERT_FF = 512
SHARED_FF = 512
ROUTED_SCALE = 2.5
DISPATCH_BLOCK = 64

DEEPNORM_ALPHA = (2.0 * DEPTH) ** 0.25
DEEPNORM_BETA = (8.0 * DEPTH) ** -0.25
LN_EPS = 1e-5
RMS_EPS = 1e-6
N_MOD = 6

kernel_name = "hybrid_dilated_swa_gla_moe_deepnorm_adaln"


def _layer_norm(x, g, b):
    xf = x.astype(jnp.float32)
    mu = xf.mean(-1, keepdims=True)
    var = jnp.square(xf - mu).mean(-1, keepdims=True)
    return ((xf - mu) * lax.rsqrt(var + LN_EPS)).astype(x.dtype) * g + b


def _dilated_window_attention(q, k, v, dilation, n_steps):
    b, s, h, dh = q.shape
    L = s // dilation
    nb = -(-L // SWA_BLOCK)
    lp = nb * SWA_BLOCK

    def to_residue(t):
        t = t.reshape(b, L, dilation, h, dh).transpose(0, 2, 3, 1, 4)
        t = jnp.pad(t, ((0, 0), (0, 0), (0, 0), (0, lp - L), (0, 0)))
        return t.reshape(b, dilation, h, nb, SWA_BLOCK, dh)

    def with_prev(t):
        prev = jnp.pad(t[:, :, :, :-1], ((0, 0), (0, 0), (0, 0), (1, 0), (0, 0), (0, 0)))
        return jnp.concatenate([prev, t], axis=4)

    qr, kr, vr = to_residue(q), to_residue(k), to_residue(v)
    kc, vc = with_prev(kr), with_prev(vr)
    scores = jnp.einsum('brhnqd,brhnkd->brhnqk', qr, kc).astype(jnp.float32) * (dh ** -0.5)
    qi = jnp.arange(SWA_BLOCK)[:, None]
    kj = jnp.arange(2 * SWA_BLOCK)[None, :]
    dist = qi + SWA_BLOCK - kj
    key_pos = jnp.arange(nb)[:, None, None] * SWA_BLOCK - SWA_BLOCK + kj[None]
    mask = (dist >= 0)[None] & (dist <= n_steps)[None] & (key_pos >= 0)
    scores = jnp.where(mask, scores, -jnp.inf)
    m = scores.max(-1, keepdims=True)
    p = jnp.exp(scores - m)
    l = p.sum(-1, keepdims=True)
    o = jnp.einsum('brhnqk,brhnkd->brhnqd', (p / l).astype(v.dtype), vc)
    lse = (m + jnp.log(l))[..., 0]
    o = o.reshape(b, dilation, h, lp, dh)[:, :, :, :L].transpose(0, 3, 1, 2, 4).reshape(b, s, h, dh)
    lse = lse.reshape(b, dilation, h, lp)[..., :L].transpose(0, 3, 1, 2).reshape(b, s, h)
    return o, lse


def _gla_chunked(q, k, v, log_a):
    b, s, h, dk = q.shape
    dv = v.shape[-1]
    n = s // GLA_CHUNK

    def chunks(t):
        return t.reshape(b, n, GLA_CHUNK, h, t.shape[-1]).transpose(1, 0, 3, 2, 4).astype(jnp.float32)

    qc, kc, vc, gc = chunks(q * (dk ** -0.5)), chunks(k), chunks(v), chunks(log_a)
    causal = jnp.tril(jnp.ones((GLA_CHUNK, GLA_CHUNK), bool))[:, :, None]

    def step(state, inp):
        qb, kb, vb, gb = inp
        cum = jnp.cumsum(gb, axis=2)
        total = cum[:, :, -1:]
        o_inter = jnp.einsum('bhck,bhkv->bhcv', qb * jnp.exp(cum), state)
        diff = cum[:, :, :, None, :] - cum[:, :, None, :, :]
        decay = jnp.exp(jnp.where(causal, diff, -jnp.inf))
        attn = jnp.einsum('bhik,bhjk,bhijk->bhij', qb, kb, decay)
        o_intra = jnp.einsum('bhij,bhjv->bhiv', attn, vb)
        new_state = jnp.exp(total)[:, :, 0, :, None] * state + jnp.einsum(
            'bhck,bhcv->bhkv', kb * jnp.exp(total - cum), vb)
        return new_state, o_inter + o_intra

    state0 = jnp.zeros((b, h, dk, dv), jnp.float32)
    _, o = lax.scan(step, state0, (qc, kc, vc, gc))
    return o.transpose(1, 0, 3, 2, 4).reshape(b, s, h, dv)


def _mixer(h, w_in, w_gla_gate, b_gla_gate, gla_norm_w, w_out):
    b, s, _ = h.shape
    proj = h @ w_in
    qa, ka, va, qg, kg, vg, rg, zg = jnp.split(proj, IN_SPLITS, axis=-1)

    qa = qa.reshape(b, s, SWA_HEADS, SWA_HEAD_DIM)
    ka = ka.reshape(b, s, SWA_HEADS, SWA_HEAD_DIM)
    va = va.reshape(b, s, SWA_HEADS, SWA_HEAD_DIM)
    outs, lses = [], []
    for window, dilation in DILATED_PATTERNS:
        o, lse = _dilated_window_attention(qa, ka, va, dilation, window // dilation)
        outs.append(o)
        lses.append(lse)
    mix_w = jax.nn.softmax(jnp.stack(lses, 0), axis=0)
    o_swa = jnp.einsum('pbsh,pbshd->bshd', mix_w, jnp.stack(outs, 0).astype(jnp.float32))
    o_swa = o_swa.reshape(b, s, SWA_WIDTH).astype(h.dtype)

    log_a = jax.nn.log_sigmoid((zg @ w_gla_gate + b_gla_gate).astype(jnp.float32)) / GLA_GATE_TEMP
    o_gla = _gla_chunked(qg.reshape(b, s, GLA_HEADS, GLA_KEY_DIM),
                         kg.reshape(b, s, GLA_HEADS, GLA_KEY_DIM),
                         vg.reshape(b, s, GLA_HEADS, GLA_VALUE_DIM),
                         log_a.reshape(b, s, GLA_HEADS, GLA_KEY_DIM))
    o_gla = o_gla * lax.rsqrt(jnp.square(o_gla).mean(-1, keepdims=True) + RMS_EPS)
    o_gla = o_gla.astype(h.dtype) * gla_norm_w
    o_gla = o_gla.reshape(b, s, GLA_VALUE_WIDTH) * jax.nn.silu(rg)

    return jnp.concatenate([o_swa, o_gla], axis=-1) @ w_out


def _route(h, w_router, router_bias):
    t = h.shape[0]
    scores = jax.nn.sigmoid(h.astype(jnp.float32) @ w_router.astype(jnp.float32))
    sel = scores + router_bias.astype(jnp.float32)
    grp = sel.reshape(t, N_GROUPS, N_EXPERTS // N_GROUPS)
    grp_score = lax.top_k(grp, 2)[0].sum(-1)
    _, top_groups = lax.top_k(grp_score, TOPK_GROUPS)
    group_mask = jax.nn.one_hot(top_groups, N_GROUPS, dtype=jnp.float32).sum(1) > 0
    expert_mask = jnp.repeat(group_mask, N_EXPERTS // N_GROUPS, axis=1)
    _, idx = lax.top_k(jnp.where(expert_mask, sel, -jnp.inf), TOP_K)
    w = jnp.take_along_axis(scores, idx, axis=1)
    w = w / w.sum(-1, keepdims=True) * ROUTED_SCALE
    return idx, w.astype(h.dtype)


def _routed_experts(h, idx, w, w_gate, w_up, w_down):
    t, d = h.shape
    a = t * TOP_K
    e_flat = idx.reshape(a)
    tok_flat = (jnp.arange(a, dtype=jnp.int32) // TOP_K)
    w_flat = w.reshape(a)
    order = jnp.argsort(e_flat)
    e_s, tok_s, w_s = e_flat[order], tok_flat[order], w_flat[order]
    counts = jnp.bincount(e_flat, length=N_EXPERTS)
    padded = (counts + DISPATCH_BLOCK - 1) // DISPATCH_BLOCK * DISPATCH_BLOCK
    starts = jnp.cumsum(counts) - counts
    pends = jnp.cumsum(padded)
    pstarts = pends - padded
    dest = pstarts[e_s] + (jnp.arange(a) - starts[e_s])
    n_rows = a + N_EXPERTS * DISPATCH_BLOCK
    n_blocks = n_rows // DISPATCH_BLOCK
    row_tok = jnp.full((n_rows,), t, jnp.int32).at[dest].set(tok_s)
    row_w = jnp.zeros((n_rows,), h.dtype).at[dest].set(w_s)
    block_e = jnp.minimum(
        jnp.searchsorted(pends, jnp.arange(n_blocks) * DISPATCH_BLOCK, side='right'), N_EXPERTS - 1)
    h_pad = jnp.concatenate([h, jnp.zeros((1, d), h.dtype)], axis=0)

    def block_fn(args):
        toks, wts, e = args
        xb = h_pad[toks]
        act = jax.nn.silu(xb @ w_gate[e]) * (xb @ w_up[e])
        return (act @ w_down[e]) * wts[:, None]

    y = lax.map(block_fn, (row_tok.reshape(n_blocks, DISPATCH_BLOCK),
                           row_w.reshape(n_blocks, DISPATCH_BLOCK), block_e))
    return jax.ops.segment_sum(y.reshape(n_rows, d), row_tok, num_segments=t + 1)[:t]


def _moe(h, w_router, router_bias, w_sh_gate, w_sh_up, w_sh_down, w_exp_gate, w_exp_up, w_exp_down):
    idx, w = _route(h, w_router, router_bias)
    shared = (jax.nn.silu(h @ w_sh_gate) * (h @ w_sh_up)) @ w_sh_down
    return shared + _routed_experts(h, idx, w, w_exp_gate, w_exp_up, w_exp_down)


def setup_inputs(seed: int = 0) -> dict:
    key = jax.random.key(seed)
    ks = jax.random.split(key, 24)
    f32 = jnp.float32
    D = D_MODEL

    def nrm(k, shape, scale):
        return jax.random.normal(k, shape, f32) * scale

    col_scale = jnp.concatenate([
        jnp.ones((2 * SWA_WIDTH,), f32), jnp.full((SWA_WIDTH,), DEEPNORM_BETA, f32),
        jnp.ones((2 * GLA_KEY_WIDTH,), f32), jnp.full((GLA_VALUE_WIDTH,), DEEPNORM_BETA, f32),
        jnp.ones((GLA_VALUE_WIDTH + GLA_GATE_RANK,), f32)])
    return {
        "x": nrm(ks[0], (BATCH, SEQ, D), 1.0),
        "c": nrm(ks[1], (BATCH, D), 1.0),
        "ln_in_g": 1.0 + nrm(ks[2], (D,), 0.02),
        "ln_in_b": nrm(ks[3], (D,), 0.02),
        "w_ada": nrm(ks[4], (DEPTH, D, N_MOD * D), 0.5 * D ** -0.5),
        "b_ada": nrm(ks[5], (DEPTH, N_MOD * D), 0.02),
        "w_in": nrm(ks[6], (DEPTH, D, IN_WIDTH), D ** -0.5) * col_scale,
        "w_gla_gate": nrm(ks[7], (DEPTH, GLA_GATE_RANK, GLA_KEY_WIDTH), GLA_GATE_RANK ** -0.5),
        "b_gla_gate": nrm(ks[8], (DEPTH, GLA_KEY_WIDTH), 0.1),
        "gla_norm_w": 1.0 + nrm(ks[9], (DEPTH, GLA_VALUE_DIM), 0.02),
        "w_out": nrm(ks[10], (DEPTH, MIX_WIDTH, D), DEEPNORM_BETA * MIX_WIDTH ** -0.5),
        "ln1_g": 1.0 + nrm(ks[11], (DEPTH, D), 0.02),
        "ln1_b": nrm(ks[12], (DEPTH, D), 0.02),
        "w_router": nrm(ks[13], (DEPTH, D, N_EXPERTS), D ** -0.5),
        "router_bias": nrm(ks[14], (DEPTH, N_EXPERTS), 0.01),
        "w_sh_gate": nrm(ks[15], (DEPTH, D, SHARED_FF), D ** -0.5),
        "w_sh_up": nrm(ks[16], (DEPTH, D, SHARED_FF), D ** -0.5),
        "w_sh_down": nrm(ks[17], (DEPTH, SHARED_FF, D), DEEPNORM_BETA * SHARED_FF ** -0.5),
        "w_exp_gate": nrm(ks[18], (DEPTH, N_EXPERTS, D, EXPERT_FF), D ** -0.5),
        "w_exp_up": nrm(ks[19], (DEPTH, N_EXPERTS, D, EXPERT_FF), D ** -0.5),
        "w_exp_down": nrm(ks[20], (DEPTH, N_EXPERTS, EXPERT_FF, D), DEEPNORM_BETA * EXPERT_FF ** -0.5),
        "ln2_g": 1.0 + nrm(ks[21], (DEPTH, D), 0.02),
        "ln2_b": nrm(ks[22], (DEPTH, D), 0.02),
    }


def reference(x, c, ln_in_g, ln_in_b, w_ada, b_ada, w_in, w_gla_gate, b_gla_gate, gla_norm_w, w_out,
              ln1_g, ln1_b, w_router, router_bias, w_sh_gate, w_sh_up, w_sh_down,
              w_exp_gate, w_exp_up, w_exp_down, ln2_g, ln2_b):
    b, s, d = x.shape
    x = _layer_norm(x, ln_in_g, ln_in_b)
    cond = jax.nn.silu(c)
    for l in range(DEPTH):
        mod = cond @ w_ada[l] + b_ada[l]
        sh1, sc1, g1, sh2, sc2, g2 = [m[:, None, :] for m in jnp.split(mod, N_MOD, axis=-1)]
        h = x * (1.0 + sc1) + sh1
        mix = _mixer(h, w_in[l], w_gla_gate[l], b_gla_gate[l], gla_norm_w[l], w_out[l])
        x = _layer_norm(DEEPNORM_ALPHA * x + g1 * mix, ln1_g[l], ln1_b[l])
        h = (x * (1.0 + sc2) + sh2).reshape(b * s, d)
        ffn = _moe(h, w_router[l], router_bias[l], w_sh_gate[l], w_sh_up[l], w_sh_down[l],
                   w_exp_gate[l], w_exp_up[l], w_exp_down[l]).reshape(b, s, d)
        x = _layer_norm(DEEPNORM_ALPHA * x + g2 * ffn, ln2_g[l], ln2_b[l])
    return x
```

```python
import numpy as np
from contextlib import ExitStack
import concourse.bass as bass
import concourse.mybir as mybir
from concourse.bass_utils import run_bass_kernel_spmd

F32 = mybir.dt.float32
BF16 = mybir.dt.bfloat16
AF = mybir.ActivationFunctionType
ALU = mybir.AluOpType
AX = mybir.AxisListType

D = 2048
KC = 16
HP_ORDER = [0, 1, 2, 3]
NBLK = 320
I32 = mybir.dt.int32
U32 = mybir.dt.uint32
P1_ORDER = list(range(8))
SEQ = 2048
OWN = 1024
NE = 256
FF = 512
IN_W = 6160
ALPHA = 2.0 ** 0.25
NMASK = 19


class _Sem:
    def __init__(self, h, name):
        self.h = h
        self.name = name
        self.count = 0


class _Eng:
    def __init__(self, h, sem, name):
        self.h = h
        self.sem = sem
        self.name = name
        self.seen = {}


class Buf:
    __slots__ = ("name", "w", "r")

    def __init__(self, name):
        self.name = name
        self.w = None
        self.r = {}


class Sched:
    def __init__(self, nc, es, n_dma_sems=24):
        self.nc = nc
        mk = lambda n: _Sem(es.enter_context(nc.semaphore(n)), n)
        self.pe = _Eng(nc.tensor, mk("s_pe"), "pe")
        self.act = _Eng(nc.scalar, mk("s_act"), "act")
        self.dve = _Eng(nc.vector, mk("s_dve"), "dve")
        self.pool = _Eng(nc.gpsimd, mk("s_pool"), "pool")
        self.sp = _Eng(nc.sync, mk("s_sp"), "sp")
        self.dsems = [mk(f"s_dma{i}") for i in range(n_dma_sems)]
        self.dnext = 0

    def _wait(self, eng, deps):
        best = {}
        for d in deps:
            if d is None:
                continue
            s, v = d
            if best.get(s, 0) < v:
                best[s] = v
        for s, v in best.items():
            if s is eng.sem and eng is self.pe:
                continue
            if eng.seen.get(s, 0) < v:
                eng.h.wait_ge(s.h, v)
                eng.seen[s] = v

    def _deps(self, reads, writes):
        deps = []
        for b in reads:
            deps.append(b.w)
        for b in writes:
            deps.append(b.w)
            deps.extend(b.r.items())
        return deps

    def _mark(self, ev, reads, writes):
        for b in reads:
            if b.r.get(ev[0], 0) < ev[1]:
                b.r[ev[0]] = ev[1]
        for b in writes:
            b.w = ev
            b.r = {}

    def op(self, eng, fn, reads=(), writes=(), signal=True):
        self._wait(eng, self._deps(reads, writes))
        ins = fn()
        if signal:
            eng.sem.count += 1
            ins.then_inc(eng.sem.h, 1)
            ev = (eng.sem, eng.sem.count)
        else:
            ev = (eng.sem, eng.sem.count + 1)
        self._mark(ev, reads, writes)
        return ins

    def dma(self, q, out, in_, reads=(), writes=(), **kw):
        s = self.dsems[self.dnext]
        self.dnext = (self.dnext + 1) % len(self.dsems)
        deps = self._deps(reads, writes)
        deps.append((s, s.count))
        self._wait(q, deps)
        ins = q.h.dma_start(out=out, in_=in_, **kw)
        s.count += 16
        ins.then_inc(s.h, 16)
        ev = (s, s.count)
        self._mark(ev, reads, writes)
        return ev

    def dma_gather(self, out, in_, idx_ap, element_offset, reads=(), writes=()):
        q = self.pool
        s = self.dsems[self.dnext]
        self.dnext = (self.dnext + 1) % len(self.dsems)
        deps = self._deps(reads, writes)
        deps.append((s, s.count))
        self._wait(q, deps)
        ins = q.h.indirect_dma_start(out=out, out_offset=None, in_=in_,
                                     in_offset=bass.IndirectOffsetOnAxis(ap=idx_ap, axis=0),
                                     element_offset=element_offset)
        s.count += 16
        ins.then_inc(s.h, 16)
        ev = (s, s.count)
        self._mark(ev, reads, writes)
        return ev

    def wait_all(self, eng, bufs):
        self._wait(eng, self._deps(bufs, bufs))

    def barrier(self):
        engs = (self.pe, self.act, self.dve, self.pool, self.sp)
        for e in engs:
            deps = [(x.sem, x.sem.count) for x in engs if x is not e] + [(s, s.count) for s in self.dsems]
            self._wait(e, deps)


def build_program(n_exp=NE + 1, debug=False, nblk=NBLK):
    nc = bass.Bass("TRN2", target_bir_lowering=False)
    dt = nc.dram_tensor

    xc = dt("xc", [SEQ, D], F32, kind="ExternalInput").ap()
    cfm = dt("cfm", [128, KC], F32, kind="ExternalInput").ap()
    flag = dt("flag", [128, 1], F32, kind="ExternalInput").ap()
    fmv = dt("fmv", [128, 4 * KC], F32, kind="ExternalInput").ap()
    bcv = dt("bcv", [128, 6 * D + 512], F32, kind="ExternalInput").ap()
    w_ada = dt("w_ada", [24, 128, KC, 512], F32, kind="ExternalInput").ap()
    b_ada = dt("b_ada", [1, 6 * D], F32, kind="ExternalInput").ap()
    w_in = dt("w_in", [49, 128, KC, 128], F32, kind="ExternalInput").ap()
    wgate = dt("wgate", [17, 512], F32, kind="ExternalInput").ap()
    w_out = dt("w_out", [128, KC, D], F32, kind="ExternalInput").ap()
    w_router = dt("w_router", [128, KC, NE], F32, kind="ExternalInput").ap()
    wg_all = dt("wg_all", [n_exp, 128, KC, FF], F32, kind="ExternalInput").ap()
    wu_all = dt("wu_all", [n_exp, 128, KC, FF], F32, kind="ExternalInput").ap()
    wd_all = dt("wd_all", [n_exp, 128, 4, D], F32, kind="ExternalInput").ap()
    masks_d = dt("masks", [128, NMASK * 128], F32, kind="ExternalInput").ap()
    tri_d = dt("tri", [128, 9 * 128], F32, kind="ExternalInput").ap()
    out_d = dt("out", [OWN, D], F32, kind="ExternalOutput").ap()
    x1_d = dt("x1_scr", [OWN, D], F32).ap()
    if debug:
        dbg_x1 = dt("dbg_x1", [OWN, D], F32, kind="ExternalOutput").ap()
        dbg_g = dt("dbg_g", [128, 8, NE + 1], F32, kind="ExternalOutput").ap()
        dbg_small = dt("dbg_small", [128, 176], F32, kind="ExternalOutput").ap()
        dbg_rank = dt("dbg_rank", [128, 8, NE], F32, kind="ExternalOutput").ap()
        dbg_c8 = dt("dbg_c8", [128, 128], F32, kind="ExternalOutput").ap()
        dbg_eb = dt("dbg_eb", [128, 384], U32, kind="ExternalOutput").ap()
        dbg_mix = dt("dbg_mix", [128, KC, OWN], F32, kind="ExternalOutput").ap()

    es = ExitStack()
    with es:
        S = Sched(nc, es)
        pe, act, dve, pool, sp = S.pe, S.act, S.dve, S.pool, S.sp

        def sb(name, shape, dtype, stack=es):
            return stack.enter_context(nc.sbuf_tensor(name, shape, dtype))

        banks = [es.enter_context(nc.psum_tensor(f"ps{i}", [128, 512], F32)) for i in range(8)]
        pb = [Buf(f"ps{i}") for i in range(8)]

        ident = sb("ident", [128, 128], F32)
        triU = sb("triU", [128, 128], F32)
        triL = sb("triL", [128, 128], F32)
        triU_bf = sb("triU_bf", [128, 512], BF16)
        ident_bf = sb("ident_bf", [128, 128], BF16)
        triS_bf = sb("triS_bf", [128, 128], BF16)
        iota_f = sb("iota_f", [128, 128], F32)
        ones_bf = sb("ones_bf", [128, 128], BF16)
        ones_f = sb("ones_f", [1, 128], F32)
        flag_sb = sb("flag_sb", [128, 1], F32)
        fmv_sb = sb("fmv_sb", [128, 4 * KC], F32)
        cond = sb("cond", [128, KC], F32)
        mod_fm = sb("mod_fm", [128, 96], F32)
        AB = sb("AB", [128, 4 * KC], F32)
        g_bc = sb("g_bc", [128, 2 * D], F32)
        rb_bc = sb("rb_bc", [128, 512], F32)
        stats = sb("stats", [128, 16], F32)
        b_const = Buf("const")
        b_mod = Buf("mod")
        b_stats = Buf("stats")
        b_G = Buf("G")

        S.dma(sp, ident[:], tri_d[:, 0:128], writes=[b_const])
        S.dma(sp, triU[:], tri_d[:, 128:256], writes=[b_const])
        S.dma(sp, triL[:], tri_d[:, 256:384], writes=[b_const])
        S.dma(pool, triS_bf[:], tri_d[:, 384:512], writes=[b_const])
        S.dma(sp, iota_f[:], tri_d[:, 512:640], writes=[b_const])
        S.dma(sp, flag_sb[:], flag, writes=[b_const])
        S.dma(sp, fmv_sb[:], fmv, writes=[b_const])
        S.dma(sp, cond[:], cfm, writes=[b_const])
        S.dma(sp, rb_bc[:], bcv[:, 6 * D:6 * D + 512], writes=[b_const])
        S.op(dve, lambda: nc.vector.tensor_copy(out=ident_bf[:], in_=ident[:]), reads=[b_const], writes=[b_const])
        S.op(dve, lambda: nc.vector.memset(ones_bf[:], 1.0), writes=[b_const])
        S.op(dve, lambda: nc.vector.memset(ones_f[:], 1.0), writes=[b_const])
        for h4 in range(4):
            S.op(dve, lambda h4=h4: nc.vector.tensor_scalar(out=triU_bf[:, h4 * 128:(h4 + 1) * 128], in0=triU[:],
                                                            scalar1=16.0, scalar2=None, op0=ALU.mult),
                 reads=[b_const], writes=[b_const])
        S.op(act, lambda: nc.scalar.activation(out=cond[:], in_=cond[:], func=AF.Silu), reads=[b_const],
             writes=[b_const])

        with ExitStack() as ph:
            mod_row = sb("mod_row", [1, 6 * D], F32, ph)
            bada = sb("bada", [1, 6 * D], F32, ph)
            wa = [sb(f"wa{i}", [128, KC, 512], F32, ph) for i in range(2)]
            b_wa = [Buf("wa0"), Buf("wa1")]
            b_row = Buf("mod_row")
            S.dma(sp, bada[:], b_ada, writes=[b_row])
            for cch in range(24):
                i = cch % 2
                S.dma(sp, wa[i][:], w_ada[cch], writes=[b_wa[i]])
                for k in range(KC):
                    S.op(pe, lambda k=k, i=i: nc.tensor.matmul(banks[0][0:1, :], cond[:, k:k + 1], wa[i][:, k, :],
                                                                start=(k == 0), stop=(k == KC - 1)),
                         reads=[b_wa[i], b_const], writes=[pb[0]], signal=(k == KC - 1))
                S.op(dve, lambda cch=cch: nc.vector.tensor_tensor(out=mod_row[0:1, cch * 512:(cch + 1) * 512],
                                                                  in0=banks[0][0:1, :],
                                                                  in1=bada[0:1, cch * 512:(cch + 1) * 512], op=ALU.add),
                     reads=[pb[0], b_row], writes=[b_row])
            for cidx in range(96):
                S.op(pe, lambda cidx=cidx: nc.tensor.matmul(banks[1][:, cidx:cidx + 1],
                                                            mod_row[0:1, cidx * 128:(cidx + 1) * 128],
                                                            ones_f[0:1, 0:1], start=True, stop=True),
                     reads=[b_row, b_const], writes=[pb[1]], signal=(cidx == 95))
            S.op(dve, lambda: nc.vector.tensor_copy(out=mod_fm[:], in_=banks[1][:, 0:96]), reads=[pb[1]],
                 writes=[b_mod])
            for gi, base in enumerate((2 * D, 5 * D)):
                for n in range(4):
                    S.op(pe, lambda base=base, n=n: nc.tensor.matmul(banks[2][:, :], ones_f[0:1, :],
                                                                     mod_row[0:1, base + n * 512:base + (n + 1) * 512],
                                                                     start=True, stop=True),
                         reads=[b_row, b_const], writes=[pb[2]])
                    S.op(dve, lambda gi=gi, n=n: nc.vector.tensor_copy(
                        out=g_bc[:, gi * D + n * 512:gi * D + (n + 1) * 512], in_=banks[2][:, :]),
                        reads=[pb[2]], writes=[b_mod])
            for t, (sh_c, sc_c) in enumerate(((0, 16), (48, 64))):
                gcol, bcol = (0, 16) if t == 0 else (32, 48)
                S.op(dve, lambda t=t, sc_c=sc_c, gcol=gcol: nc.vector.scalar_tensor_tensor(
                    out=AB[:, t * 32:t * 32 + 16], in0=mod_fm[:, sc_c:sc_c + 16], scalar=1.0,
                    in1=fmv_sb[:, gcol:gcol + 16], op0=ALU.add, op1=ALU.mult),
                    reads=[b_mod, b_const], writes=[b_mod])
                S.op(dve, lambda t=t, sc_c=sc_c, bcol=bcol: nc.vector.scalar_tensor_tensor(
                    out=AB[:, t * 32 + 16:t * 32 + 32], in0=mod_fm[:, sc_c:sc_c + 16], scalar=1.0,
                    in1=fmv_sb[:, bcol:bcol + 16], op0=ALU.add, op1=ALU.mult),
                    reads=[b_mod, b_const], writes=[b_mod])
                S.op(dve, lambda t=t, sh_c=sh_c: nc.vector.tensor_tensor(
                    out=AB[:, t * 32 + 16:t * 32 + 32], in0=AB[:, t * 32 + 16:t * 32 + 32],
                    in1=mod_fm[:, sh_c:sh_c + 16], op=ALU.add), reads=[b_mod], writes=[b_mod])
            S.barrier()

        def cast_dma(dst3, src3, nk, ncols, bw):
            step = max(1, 2048 // ncols)
            for k0 in range(0, nk, step):
                S.dma(pool, dst3[:, k0:k0 + step, :], src3[:, k0:k0 + step, :], writes=[bw])

        def layer_norm_stats(xt, b_xt, mv, b_mv, st6, b_st6):
            for q in range(4):
                S.op(dve, lambda q=q: nc.vector.bn_stats(out=st6[:, q * 6:(q + 1) * 6], in_=xt[:, q * 512:(q + 1) * 512]),
                     reads=[b_xt], writes=[b_st6])
            S.op(dve, lambda: nc.vector.bn_aggr(out=mv, in_=st6[:, 0:24]), reads=[b_st6], writes=[b_mv])

        def rstd_inplace(ap, b, eps):
            S.op(dve, lambda: nc.vector.tensor_scalar_add(out=ap, in0=ap, scalar1=eps), reads=[b], writes=[b])
            S.op(act, lambda: nc.scalar.activation(out=ap, in_=ap, func=AF.Ln), reads=[b], writes=[b])
            S.op(act, lambda: nc.scalar.activation(out=ap, in_=ap, func=AF.Exp, scale=-0.5), reads=[b], writes=[b])

        def ln_normalize(x_ap, b_x, mvt, b_mvt):
            rstd_inplace(mvt[:, 1:2], b_mvt, 1e-5)
            S.op(dve, lambda: nc.vector.scalar_tensor_tensor(out=mvt[:, 2:3], in0=mvt[:, 0:1], scalar=-1.0,
                                                             in1=mvt[:, 1:2], op0=ALU.mult, op1=ALU.mult),
                 reads=[b_mvt], writes=[b_mvt])
            S.op(dve, lambda: nc.vector.tensor_scalar(out=x_ap, in0=x_ap, scalar1=mvt[:, 1:2], scalar2=mvt[:, 2:3],
                                                      op0=ALU.mult, op1=ALU.add), reads=[b_mvt, b_x], writes=[b_x])

        def all_wait(bufs):
            for e_ in (pe, act, dve, pool, sp):
                S.wait_all(e_, bufs)
            S.barrier()

        Gt = sb("Gt", [128, 8, NE + 1], F32)
        S.op(dve, lambda: nc.vector.memset(Gt[:], 1.0), writes=[b_G])
        mixT = sb("mixT", [128, 8, KC, 128], BF16)
        b_mix = [Buf(f"mix{k}") for k in range(KC)]

        with ExitStack() as phA:
            hT = sb("hT", [128, KC, OWN], BF16, phA)
            b_hT = [Buf(f"hT{t}") for t in range(8)]
            zgT = sb("zgT", [17, OWN], F32, phA)
            wgt = sb("wgt", [17, 512], F32, phA)
            b_zg = Buf("zgT")
            St = sb("St", [128, 4, 256], F32, phA)
            St_bf = sb("St_bf", [128, 4, 256], BF16, phA)
            b_St, b_Stbf = Buf("St"), Buf("Stbf")
            xt = sb("xt", [128, D], F32, phA)
            b_xt = Buf("xt")
            st6 = sb("st6", [128, 24], F32, phA)
            b_st6 = Buf("st6")
            mv = sb("mv", [128, 64], F32, phA)
            b_mv = Buf("mv")
            la = sb("la", [128, 512], F32, phA)
            Eq = sb("Eq", [128, 512], F32, phA)
            Er = sb("Er", [128, 512], F32, phA)
            khat = sb("khat", [128, 512], BF16, phA)
            b_la, b_Eq, b_Er, b_khat = Buf("la"), Buf("Eq"), Buf("Er"), Buf("khat")
            wch = [sb(f"wch{i}", [128, KC, 128], BF16, phA) for i in range(2)]
            b_wch = [Buf(f"wch{i}") for i in range(2)]
            wctr = [0]
            S.dma(sp, wgt[:], wgate, writes=[b_zg])
            S.op(dve, lambda: nc.vector.memset(zgT[:], 1.0), writes=[b_zg])
            S.op(dve, lambda: nc.vector.memset(St[:], 0.0), writes=[b_St])
            S.op(dve, lambda: nc.vector.memset(St_bf[:], 0.0), writes=[b_Stbf])

            def ln_pass(tile0):
                for tt in range(8):
                    t = tile0 + tt
                    S.dma(sp, xt[:], xc[t * 128:(t + 1) * 128, :], writes=[b_xt])
                    layer_norm_stats(xt, b_xt, mv[:, 4 * t:4 * t + 2], b_mv, st6, b_st6)
                    ln_normalize(xt[:], b_xt, mv[:, 4 * t:4 * t + 4], b_mv)
                    if t >= 8:
                        S.op(dve, lambda t=t: nc.vector.tensor_copy(out=stats[:, 2 * (t - 8):2 * (t - 8) + 2],
                                                                    in_=mv[:, 4 * t + 1:4 * t + 3]),
                             reads=[b_mv], writes=[b_stats])
                    for q in range(4):
                        bk = 4 + (q % 2)
                        for kk in range(4):
                            k = q * 4 + kk
                            S.op(pe, lambda k=k, kk=kk, bk=bk: nc.tensor.transpose(
                                banks[bk][:, kk * 128:(kk + 1) * 128], xt[:, k * 128:(k + 1) * 128], ident[:]),
                                reads=[b_xt, b_const], writes=[pb[bk]], signal=(kk == 3))
                        for kk in range(4):
                            k = q * 4 + kk
                            S.op(dve, lambda k=k, kk=kk, tt=tt, bk=bk: nc.vector.tensor_scalar(
                                out=hT[:, k, tt * 128:(tt + 1) * 128], in0=banks[bk][:, kk * 128:(kk + 1) * 128],
                                scalar1=AB[:, k:k + 1], scalar2=AB[:, 16 + k:17 + k], op0=ALU.mult, op1=ALU.add),
                                reads=[pb[bk], b_mod], writes=[b_hT[tt]])

            def load_w_in(chunk):
                i = wctr[0] % 2
                wctr[0] += 1
                S.dma(pool, wch[i][:], w_in[chunk], writes=[b_wch[i]])
                return wch[i], b_wch[i]

            def proj_fm(chunk, dst_fn, b_dst, rows=128):
                w, bw = load_w_in(chunk)
                for g in range(2):
                    bk = 4 + (g % 2)
                    for k in range(KC):
                        S.op(pe, lambda k=k, bk=bk, g=g, w=w: nc.tensor.matmul(
                            banks[bk][0:rows, :], w[:, k, 0:rows], hT[:, k, g * 512:(g + 1) * 512], start=(k == 0),
                            stop=(k == KC - 1)),
                            reads=[bw] + b_hT[g * 4:g * 4 + 4], writes=[pb[bk]], signal=(k == KC - 1))
                    S.op(act, lambda bk=bk, g=g: nc.scalar.copy(out=dst_fn(g), in_=banks[bk][0:rows, :]),
                         reads=[pb[bk]], writes=[b_dst])

            def proj_tm(chunk, dst_fn, b_dst_fn):
                w, bw = load_w_in(chunk)
                for tt in range(8):
                    bk = 6 + (tt % 2)
                    for k in range(KC):
                        S.op(pe, lambda k=k, bk=bk, tt=tt, w=w: nc.tensor.matmul(
                            banks[bk][:, 0:128], hT[:, k, tt * 128:(tt + 1) * 128], w[:, k, :], start=(k == 0),
                            stop=(k == KC - 1)),
                            reads=[bw, b_hT[tt]], writes=[pb[bk]], signal=(k == KC - 1))
                    if tt % 2 == 0:
                        S.op(act, lambda bk=bk, tt=tt: nc.scalar.copy(out=dst_fn(tt), in_=banks[bk][:, 0:128]),
                             reads=[pb[bk]], writes=[b_dst_fn(tt)])
                    else:
                        S.op(dve, lambda bk=bk, tt=tt: nc.vector.tensor_copy(out=dst_fn(tt), in_=banks[bk][:, 0:128]),
                             reads=[pb[bk]], writes=[b_dst_fn(tt)])

            def proj_zg(col0):
                w, bw = load_w_in(48)
                for g in range(2):
                    bk = 4 + (g % 2)
                    for k in range(KC):
                        S.op(pe, lambda k=k, bk=bk, g=g, w=w: nc.tensor.matmul(
                            banks[bk][0:16, :], w[:, k, 0:16], hT[:, k, g * 512:(g + 1) * 512], start=(k == 0),
                            stop=(k == KC - 1)), reads=[bw] + b_hT[g * 4:g * 4 + 4], writes=[pb[bk]],
                            signal=(k == KC - 1))
                    S.op(act, lambda bk=bk, g=g: nc.scalar.copy(out=zgT[0:16, g * 512:(g + 1) * 512],
                                                                in_=banks[bk][0:16, :]), reads=[pb[bk]], writes=[b_zg])

            def gla_gates(c):
                S.op(pe, lambda: nc.tensor.matmul(banks[0][:, :], zgT[0:17, (c % 8) * 128:(c % 8 + 1) * 128], wgt[0:17, :],
                                                  start=True, stop=True), reads=[b_zg], writes=[pb[0]])
                S.op(act, lambda: nc.scalar.activation(out=la[:], in_=banks[0][:, :], func=AF.Sigmoid),
                     reads=[pb[0]], writes=[b_la])
                S.op(act, lambda: nc.scalar.activation(out=la[:], in_=la[:], func=AF.Ln), reads=[b_la], writes=[b_la])
                S.op(pe, lambda: nc.tensor.matmul(banks[0][:, :], triL[:], la[:], start=True, stop=True),
                     reads=[b_const, b_la], writes=[pb[0]])
                S.op(act, lambda: nc.scalar.activation(out=Er[:], in_=banks[0][:, :], func=AF.Exp), reads=[pb[0]],
                     writes=[b_Er])
                for h in range(4):
                    S.op(pe, lambda h=h: nc.tensor.matmul(banks[1][:, h * 128:(h + 1) * 128],
                                                          la[:, h * 128:(h + 1) * 128], triU[:], start=True,
                                                          stop=True), reads=[b_const, b_la], writes=[pb[1]],
                         signal=(h == 3))
                S.op(act, lambda: nc.scalar.activation(out=Eq[:], in_=banks[1][:, :], func=AF.Exp),
                     reads=[pb[1]], writes=[b_Eq])

            def gla_state_update(kg_ap, b_kgc, vg_fn, b_vgc, use_flag):
                if use_flag:
                    S.op(dve, lambda: nc.vector.scalar_tensor_tensor(
                        out=khat[:], in0=kg_ap, scalar=flag_sb[:, 0:1], in1=Er[:], op0=ALU.mult, op1=ALU.mult),
                        reads=[b_kgc, b_Er, b_const], writes=[b_khat])
                else:
                    S.op(dve, lambda: nc.vector.tensor_tensor(out=khat[:], in0=kg_ap, in1=Er[:], op=ALU.mult),
                         reads=[b_kgc, b_Er], writes=[b_khat])
                for h in range(4):
                    bk = 6 + h // 2
                    cs = (h % 2) * 256
                    S.op(pe, lambda h=h, bk=bk, cs=cs: nc.tensor.matmul(
                        banks[bk][:, cs:cs + 256], khat[:, h * 128:(h + 1) * 128], vg_fn(h), start=True, stop=True),
                        reads=[b_khat, b_vgc], writes=[pb[bk]], signal=(h % 2 == 1))
                for h in range(4):
                    bk = 6 + h // 2
                    cs = (h % 2) * 256
                    S.op(dve, lambda h=h, bk=bk, cs=cs: nc.vector.scalar_tensor_tensor(
                        out=St[:, h, :], in0=St[:, h, :], scalar=Eq[:, h * 128 + 127:h * 128 + 128],
                        in1=banks[bk][:, cs:cs + 256], op0=ALU.mult, op1=ALU.add),
                        reads=[b_St, b_Eq, pb[bk]], writes=[b_St])
                S.op(act, lambda: nc.scalar.copy(out=St_bf[:], in_=St[:]), reads=[b_St], writes=[b_Stbf])

            S1 = ExitStack()
            KTp = sb("KTp", [128, 8, OWN], BF16, S1)
            Vp = sb("Vp", [128, 8, 1024], BF16, S1)
            b_KTp = [Buf(f"KTp{h}") for h in range(8)]
            b_Vp = [Buf(f"Vp{h}") for h in range(8)]
            ln_pass(0)
            with ExitStack() as S2:
                kgp = sb("kgp", [128, 8, 512], BF16, S2)
                vgp = sb("vgp", [128, 8, 1024], BF16, S2)
                b_kgp = [Buf(f"kgp{t}") for t in range(8)]
                b_vgp = [Buf(f"vgp{t}") for t in range(8)]
                for h in P1_ORDER:
                    proj_fm(8 + h, lambda g, h=h: KTp[:, h, g * 512:(g + 1) * 512], b_KTp[h])
                    proj_tm(16 + h, lambda tt, h=h: Vp[:, tt, h * 128:(h + 1) * 128], lambda tt, h=h: b_Vp[h])
                for h in range(4):
                    proj_tm(28 + h, lambda tt, h=h: kgp[:, tt, h * 128:(h + 1) * 128], lambda tt: b_kgp[tt])
                for cch in range(8):
                    proj_tm(32 + cch, lambda tt, cch=cch: vgp[:, tt, cch * 128:(cch + 1) * 128], lambda tt: b_vgp[tt])
                proj_zg(0)
                for c in range(8):
                    gla_gates(c)
                    gla_state_update(kgp[:, c, :], b_kgp[c], lambda h, c=c: vgp[:, c, h * 256:(h + 1) * 256], b_vgp[c],
                                     True)
                all_wait(b_kgp + b_vgp + [b_khat])

            ln_pass(8)
            proj_zg(OWN)
            with ExitStack() as SW:
                mask_bf = sb("mask_bf", [128, NMASK * 128], BF16, SW)
                maskp_bf = sb("maskp_bf", [128, NMASK * 128], BF16, SW)
                b_mask = Buf("mask")
                S.dma(pool, mask_bf[:, 0:1280], masks_d[:, 0:1280], writes=[b_mask])
                S.dma(pool, mask_bf[:, 1280:NMASK * 128], masks_d[:, 1280:NMASK * 128], writes=[b_mask])
                S.op(dve, lambda: nc.vector.tensor_scalar(out=maskp_bf[:], in0=mask_bf[:], scalar1=flag_sb[:, 0:1],
                                                          scalar2=None, op0=ALU.mult), reads=[b_mask, b_const],
                     writes=[b_mask])
                QT = sb("QT", [128, 2, OWN], BF16, SW)
                KTo = sb("KTo", [128, 2, OWN], BF16, SW)
                Vo = sb("Vo", [128, 8, 256], BF16, SW)
                Pt = [sb(f"Pt{i}", [128, 512], BF16, SW) for i in range(2)]
                rl = sb("rl", [128, 512], F32, SW)
                b_QT, b_KTo, b_Vo = Buf("QT"), Buf("KTo"), Buf("Vo")
                b_Pt = [Buf("Pt0"), Buf("Pt1")]
                b_rl = Buf("rl")
                scale = 128.0 ** -0.5
                it = 0
                for hp in HP_ORDER:
                    for hh in range(2):
                        h = 2 * hp + hh
                        proj_fm(h, lambda g, hh=hh: QT[:, hh, g * 512:(g + 1) * 512], b_QT)
                        proj_fm(8 + h, lambda g, hh=hh: KTo[:, hh, g * 512:(g + 1) * 512], b_KTo)
                        proj_tm(16 + h, lambda tt, hh=hh: Vo[:, tt, hh * 128:(hh + 1) * 128], lambda tt: b_Vo)
                    for hh in range(2):
                        h = 2 * hp + hh
                        for qg in range(2):
                            n0 = 8 + 4 * qg
                            nkb = n0 + 4
                            for j in range(nkb):
                                sbk = it % 2
                                pi = it % 2
                                it += 1
                                if j < 8:
                                    kap, vap, bk_, bv_ = (KTp[:, h, j * 128:(j + 1) * 128],
                                                          Vp[:, j, h * 128:(h + 1) * 128], b_KTp[h], b_Vp[h])
                                    msk = maskp_bf
                                else:
                                    kap, vap, bk_, bv_ = (KTo[:, hh, (j - 8) * 128:(j - 7) * 128],
                                                          Vo[:, j - 8, hh * 128:(hh + 1) * 128], b_KTo, b_Vo)
                                    msk = mask_bf
                                S.op(pe, lambda kap=kap, hh=hh, qg=qg, sbk=sbk: nc.tensor.matmul(
                                    banks[sbk][:, :], kap, QT[:, hh, qg * 512:(qg + 1) * 512], start=True, stop=True),
                                    reads=[bk_, b_QT], writes=[pb[sbk]])
                                S.op(act, lambda sbk=sbk, pi=pi: nc.scalar.activation(
                                    out=Pt[pi][:], in_=banks[sbk][:, :], func=AF.Exp, scale=scale),
                                    reads=[pb[sbk]], writes=[b_Pt[pi]])
                                moff = (n0 - j + 3) * 128
                                S.op(dve, lambda pi=pi, moff=moff, msk=msk: nc.vector.tensor_tensor(
                                    out=Pt[pi][:], in0=Pt[pi][:], in1=msk[:, moff:moff + 512], op=ALU.mult),
                                    reads=[b_Pt[pi], b_mask], writes=[b_Pt[pi]])
                                S.op(pe, lambda vap=vap, j=j, pi=pi, nkb=nkb: nc.tensor.matmul(
                                    banks[2][:, :], vap, Pt[pi][:], start=(j == 0), stop=(j == nkb - 1)),
                                    reads=[bv_, b_Pt[pi]], writes=[pb[2]], signal=False)
                                S.op(pe, lambda j=j, pi=pi, nkb=nkb: nc.tensor.matmul(
                                    banks[3][:, :], ones_bf[:], Pt[pi][:], start=(j == 0), stop=(j == nkb - 1)),
                                    reads=[b_const, b_Pt[pi]], writes=[pb[3]], signal=True)
                            S.op(dve, lambda: nc.vector.reciprocal(out=rl[:], in_=banks[3][:, :]), reads=[pb[3]],
                                 writes=[b_rl])
                            S.op(dve, lambda h=h, qg=qg: nc.vector.tensor_tensor(
                                out=mixT[:, 4 * qg:4 * qg + 4, h, :], in0=banks[2][:, :].rearrange("p (a b) -> p a b", a=4),
                                in1=rl[:].rearrange("p (a b) -> p a b", a=4), op=ALU.mult),
                                reads=[pb[2], b_rl], writes=[b_mix[h]])
                all_wait([b_QT, b_KTo, b_Vo, b_rl, b_mask] + b_Pt + b_KTp + b_Vp)
            S1.close()

            with ExitStack() as SG:
                qgT = sb("qgT", [128, 4, OWN], BF16, SG)
                kgT = sb("kgT", [128, 4, OWN], BF16, SG)
                kgo = sb("kgo", [128, 8, 512], BF16, SG)
                vgo = sb("vgo", [128, 8, 1024], BF16, SG)
                rg = sb("rg", [128, 8, 1024], BF16, SG)
                Ek = la
                qtl = sb("qtl", [128, 512], BF16, SG)
                ktl = sb("ktl", [128, 512], BF16, SG)
                ATm = sb("ATm", [128, 512], BF16, SG)
                og = sb("og", [128, 4, 256], F32, SG)
                ogb = sb("ogb", [128, 1024], BF16, SG)
                sg = sb("sg", [128, 4, 256], BF16, SG)
                ss = sb("ss", [128, 8], F32, SG)
                b_qgT, b_kgT = Buf("qgT"), Buf("kgT")
                b_kgo = [Buf(f"kgo{t}") for t in range(8)]
                b_vgo = [Buf(f"vgo{t}") for t in range(8)]
                b_rg = [Buf(f"rg{t}") for t in range(8)]
                b_Ek, b_qtl, b_ktl, b_ATm, b_og, b_ogb, b_sg, b_ss = (Buf("Ek"), Buf("qtl"), Buf("ktl"), Buf("ATm"),
                                                                      Buf("og"), Buf("ogb"), Buf("sg"), Buf("ss"))
                junk = xt[:, 0:1024].rearrange("p (h v) -> p h v", h=4)
                for h in range(4):
                    proj_fm(24 + h, lambda g, h=h: qgT[:, h, g * 512:(g + 1) * 512], b_qgT)
                    proj_fm(28 + h, lambda g, h=h: kgT[:, h, g * 512:(g + 1) * 512], b_kgT)
                    proj_tm(28 + h, lambda tt, h=h: kgo[:, tt, h * 128:(h + 1) * 128], lambda tt: b_kgo[tt])
                for cch in range(8):
                    proj_tm(32 + cch, lambda tt, cch=cch: vgo[:, tt, cch * 128:(cch + 1) * 128], lambda tt: b_vgo[tt])
                    proj_tm(40 + cch, lambda tt, cch=cch: rg[:, tt, cch * 128:(cch + 1) * 128], lambda tt: b_rg[tt])
                for co in range(8):
                    c = 8 + co
                    gla_gates(c)
                    S.op(act, lambda: nc.scalar.activation(out=Ek[:], in_=banks[1][:, :], func=AF.Exp, scale=-1.0),
                         reads=[pb[1]], writes=[b_Ek, b_la])
                    for h in range(4):
                        S.op(dve, lambda h=h, co=co: nc.vector.scalar_tensor_tensor(
                            out=qtl[:, h * 128:(h + 1) * 128], in0=qgT[:, h, co * 128:(co + 1) * 128],
                            scalar=128.0 ** -0.5, in1=Eq[:, h * 128:(h + 1) * 128], op0=ALU.mult, op1=ALU.mult),
                            reads=[b_qgT, b_Eq], writes=[b_qtl])
                        S.op(dve, lambda h=h, co=co: nc.vector.tensor_tensor(
                            out=ktl[:, h * 128:(h + 1) * 128], in0=kgT[:, h, co * 128:(co + 1) * 128],
                            in1=Ek[:, h * 128:(h + 1) * 128], op=ALU.mult), reads=[b_kgT, b_Ek], writes=[b_ktl])
                    for h in range(4):
                        S.op(pe, lambda h=h: nc.tensor.matmul(banks[3][:, h * 128:(h + 1) * 128],
                                                              ktl[:, h * 128:(h + 1) * 128],
                                                              qtl[:, h * 128:(h + 1) * 128], start=True, stop=True),
                             reads=[b_ktl, b_qtl], writes=[pb[3]], signal=(h == 3))
                    S.op(dve, lambda: nc.vector.tensor_tensor(out=ATm[:], in0=banks[3][:, :], in1=triU_bf[:],
                                                              op=ALU.mult), reads=[pb[3], b_const], writes=[b_ATm])
                    for h in range(4):
                        bk = 4 + h // 2
                        cs = (h % 2) * 256
                        S.op(pe, lambda h=h, co=co, bk=bk, cs=cs: nc.tensor.matmul(
                            banks[bk][:, cs:cs + 256], ATm[:, h * 128:(h + 1) * 128],
                            vgo[:, co, h * 256:(h + 1) * 256], start=True, stop=False),
                            reads=[b_ATm, b_vgo[co]], writes=[pb[bk]], signal=False)
                        S.op(pe, lambda h=h, bk=bk, cs=cs: nc.tensor.matmul(
                            banks[bk][:, cs:cs + 256], qtl[:, h * 128:(h + 1) * 128], St_bf[:, h, :], start=False,
                            stop=True), reads=[b_qtl, b_Stbf], writes=[pb[bk]], signal=(h % 2 == 1))
                    for hh in range(2):
                        S.op(act, lambda hh=hh: nc.scalar.copy(out=og[:, 2 * hh:2 * hh + 2, :],
                                                               in_=banks[4 + hh][:, :].rearrange("p (h v) -> p h v", h=2)),
                             reads=[pb[4 + hh]], writes=[b_og])
                    if co < 7:
                        gla_state_update(kgo[:, co, :], b_kgo[co], lambda h, co=co: vgo[:, co, h * 256:(h + 1) * 256],
                                         b_vgo[co], False)
                    S.op(dve, lambda: nc.vector.tensor_tensor(out=junk, in0=og[:], in1=og[:], op=ALU.mult),
                         reads=[b_og], writes=[b_xt])
                    S.op(dve, lambda: nc.vector.reduce_sum(out=ss[:, 0:4], in_=junk, axis=AX.X), reads=[b_xt],
                         writes=[b_ss])
                    S.op(dve, lambda: nc.vector.tensor_scalar(out=ss[:, 0:4], in0=ss[:, 0:4], scalar1=1.0 / 256.0,
                                                              scalar2=1e-6, op0=ALU.mult, op1=ALU.add),
                         reads=[b_ss], writes=[b_ss])
                    S.op(act, lambda: nc.scalar.activation(out=ss[:, 0:4], in_=ss[:, 0:4], func=AF.Ln), reads=[b_ss],
                         writes=[b_ss])
                    S.op(act, lambda: nc.scalar.activation(out=ss[:, 0:4], in_=ss[:, 0:4], func=AF.Exp, scale=-0.5),
                         reads=[b_ss], writes=[b_ss])
                    S.op(act, lambda co=co: nc.scalar.activation(
                        out=sg[:], in_=rg[:, co, :].rearrange("p (h v) -> p h v", h=4), func=AF.Silu),
                        reads=[b_rg[co]], writes=[b_sg])
                    for h in range(4):
                        S.op(dve, lambda h=h: nc.vector.scalar_tensor_tensor(
                            out=og[:, h, :], in0=og[:, h, :], scalar=ss[:, h:h + 1], in1=rb_bc[:, 256:512],
                            op0=ALU.mult, op1=ALU.mult), reads=[b_og, b_ss, b_const], writes=[b_og])
                    S.op(dve, lambda: nc.vector.tensor_tensor(out=ogb[:].rearrange("p (h v) -> p h v", h=4), in0=og[:],
                                                              in1=sg[:], op=ALU.mult), reads=[b_og, b_sg],
                         writes=[b_ogb])
                    for half in range(2):
                        for kk in range(4):
                            k = half * 4 + kk
                            S.op(pe, lambda k=k, kk=kk: nc.tensor.matmul(
                                banks[2][:, kk * 128:(kk + 1) * 128], ogb[:, k * 128:(k + 1) * 128], ident_bf[:],
                                start=True, stop=True), reads=[b_ogb, b_const], writes=[pb[2]], signal=(kk == 3))
                        for kk in range(4):
                            k = half * 4 + kk
                            S.op(act, lambda k=k, kk=kk, co=co: nc.scalar.copy(
                                out=mixT[:, co, 8 + k, :], in_=banks[2][:, kk * 128:(kk + 1) * 128]),
                                reads=[pb[2]], writes=[b_mix[8 + k]])
                all_wait([b_qgT, b_kgT, b_Ek, b_qtl, b_ktl, b_ATm, b_og, b_ogb, b_sg, b_ss] + b_kgo + b_vgo + b_rg)
            all_wait([b_zg, b_St, b_Stbf, b_xt, b_st6, b_mv, b_la, b_Eq, b_Er, b_khat] + b_wch + b_hT)

        if debug:
            b_dd = Buf("dd")
            S.dma(sp, dbg_small[:, 0:96], mod_fm[:], reads=[b_mod], writes=[b_dd])
            S.dma(sp, dbg_small[:, 96:160], AB[:], reads=[b_mod], writes=[b_dd])
            S.dma(sp, dbg_small[:, 160:176], stats[:], reads=[b_stats], writes=[b_dd])
            for k in range(KC):
                S.dma(pool, dbg_mix[:, k, :].rearrange("p (t c) -> p t c", t=8), mixT[:, :, k, :], reads=[b_mix[k]], writes=[b_dd])
            all_wait([b_dd])
        h2tm = mixT[:].rearrange("p t k c -> p t (k c)")
        b_h2 = [Buf(f"h2tm{t}") for t in range(8)]
        rankp = sb("rankp", [128, 8, NE], F32)
        b_rank = Buf("rankp")
        code8 = sb("code8", [128, 64], F32)
        gate8 = sb("gate8", [128, 64], F32)
        eb_i = sb("eb_i", [128, 384], U32)
        b_c8 = Buf("code8")
        b_eb = Buf("eb")
        b_x1d = [Buf(f"x1d{t}") for t in range(8)]
        with ExitStack() as phB:
            wo = sb("wo", [128, KC, 512], BF16, phB)
            b_wo = Buf("wo")
            bc = sb("bc", [128, 4 * D], F32, phB)
            b_bc = Buf("bc")
            wr = sb("wr", [128, KC, NE], F32, phB)
            b_wr = Buf("wr")
            h2f = sb("h2f", [128, KC, 128], F32, phB)
            b_h2f = Buf("h2f")
            xt = sb("xtb", [128, D], F32, phB)
            b_xt = Buf("xtb")
            pre = sb("pre", [128, D], F32, phB)
            b_pre = Buf("pre")
            st6 = sb("st6b", [128, 24], F32, phB)
            b_st6 = Buf("st6b")
            mv = sb("mvb", [128, 4], F32, phB)
            b_mv = Buf("mvb")
            sc = sb("sc", [128, NE], F32, phB)
            sel = sb("sel", [128, NE], F32, phB)
            selm = sb("selm", [128, NE], F32, phB)
            m8 = sb("m8", [128, 8, 8], F32, phB)
            gsc = sb("gsc", [128, 8], F32, phB)
            gm8 = sb("gm8", [128, 8], F32, phB)
            gmask = sb("gmask", [128, 8], F32, phB)
            pen = sb("pen", [128, 8], F32, phB)
            t8 = sb("t8", [128, 8], F32, phB)
            den = sb("den", [128, 1], F32, phB)
            b_rt = Buf("route")
            Mb = sb("Mb", [128, 8, NE], BF16, phB)
            b_Mb = Buf("Mb")
            S.dma(sp, bc[:], bcv[:, 0:4 * D], writes=[b_bc])
            S.dma(sp, wr[:], w_router, writes=[b_wr])
            for t in range(8):
                S.dma(sp, xt[:], xc[OWN + t * 128:OWN + (t + 1) * 128, :], writes=[b_xt])
                S.op(dve, lambda t=t: nc.vector.tensor_scalar(out=xt[:], in0=xt[:], scalar1=stats[:, 2 * t:2 * t + 1],
                                                              scalar2=stats[:, 2 * t + 1:2 * t + 2],
                                                              op0=ALU.mult, op1=ALU.add),
                     reads=[b_stats, b_xt], writes=[b_xt])
                S.op(dve, lambda: nc.vector.tensor_tensor(out=xt[:], in0=xt[:], in1=bc[:, 0:D], op=ALU.mult),
                     reads=[b_xt, b_bc], writes=[b_xt])
                S.op(dve, lambda: nc.vector.tensor_tensor(out=xt[:], in0=xt[:], in1=bc[:, D:2 * D], op=ALU.add),
                     reads=[b_xt, b_bc], writes=[b_xt])
                for n in range(4):
                    bk = n % 2
                    cast_dma(wo, w_out[:, :, n * 512:(n + 1) * 512], KC, 512, b_wo)
                    for k in range(KC):
                        S.op(pe, lambda k=k, t=t, bk=bk: nc.tensor.matmul(
                            banks[bk][:, :], mixT[:, t, k, :], wo[:, k, :],
                            start=(k == 0), stop=(k == KC - 1)), reads=[b_mix[k], b_wo], writes=[pb[bk]],
                            signal=(k == KC - 1))
                    S.op(dve, lambda n=n, bk=bk: nc.vector.tensor_tensor(
                        out=pre[:, n * 512:(n + 1) * 512], in0=banks[bk][:, :], in1=g_bc[:, n * 512:(n + 1) * 512],
                        op=ALU.mult), reads=[pb[bk], b_mod], writes=[b_pre])
                S.op(dve, lambda: nc.vector.scalar_tensor_tensor(out=pre[:], in0=xt[:], scalar=ALPHA, in1=pre[:],
                                                                 op0=ALU.mult, op1=ALU.add),
                     reads=[b_xt, b_pre], writes=[b_pre])
                layer_norm_stats(pre, b_pre, mv[:, 0:2], b_mv, st6, b_st6)
                ln_normalize(pre[:], b_pre, mv[:, 0:4], b_mv)
                for q in range(4):
                    bk = 4 + (q % 2)
                    for kk in range(4):
                        k = q * 4 + kk
                        S.op(pe, lambda k=k, kk=kk, bk=bk: nc.tensor.transpose(
                            banks[bk][:, kk * 128:(kk + 1) * 128], pre[:, k * 128:(k + 1) * 128], ident[:]),
                            reads=[b_pre, b_const], writes=[pb[bk]], signal=(kk == 3))
                    for kk in range(4):
                        k = q * 4 + kk
                        S.op(dve, lambda k=k, kk=kk, bk=bk: nc.vector.tensor_scalar(
                            out=h2f[:, k, :], in0=banks[bk][:, kk * 128:(kk + 1) * 128], scalar1=AB[:, 32 + k:33 + k],
                            scalar2=AB[:, 48 + k:49 + k], op0=ALU.mult, op1=ALU.add), reads=[pb[bk], b_mod],
                            writes=[b_h2f])
                S.wait_all(act, b_mix)
                for q in range(4):
                    bk = 6 + (q % 2)
                    for kk in range(4):
                        k = q * 4 + kk
                        S.op(pe, lambda k=k, kk=kk, bk=bk: nc.tensor.transpose(
                            banks[bk][:, kk * 128:(kk + 1) * 128], h2f[:, k, :], ident[:]),
                            reads=[b_h2f, b_const], writes=[pb[bk]], signal=(kk == 3))
                    S.op(act, lambda t=t, q=q, bk=bk: nc.scalar.copy(out=h2tm[:, t, q * 512:(q + 1) * 512],
                                                                     in_=banks[bk][:, :]), reads=[pb[bk]],
                         writes=[b_h2[t]])
                S.op(dve, lambda: nc.vector.tensor_tensor(out=pre[:], in0=pre[:], in1=bc[:, 2 * D:3 * D], op=ALU.mult),
                     reads=[b_pre, b_bc], writes=[b_pre])
                S.op(dve, lambda: nc.vector.tensor_tensor(out=pre[:], in0=pre[:], in1=bc[:, 3 * D:4 * D], op=ALU.add),
                     reads=[b_pre, b_bc], writes=[b_pre])
                S.dma(sp, x1_d[t * 128:(t + 1) * 128, :], pre[:], reads=[b_pre], writes=[b_x1d[t]])
                if debug:
                    S.dma(sp, dbg_x1[t * 128:(t + 1) * 128, :], pre[:], reads=[b_pre], writes=[b_x1d[t]])
                for k in range(KC):
                    S.op(pe, lambda k=k: nc.tensor.matmul(banks[2][:, 0:NE], h2f[:, k, :], wr[:, k, :], start=(k == 0),
                                                          stop=(k == KC - 1)), reads=[b_h2f, b_wr], writes=[pb[2]],
                         signal=(k == KC - 1))
                S.op(act, lambda: nc.scalar.activation(out=sc[:], in_=banks[2][:, 0:NE], func=AF.Sigmoid),
                     reads=[pb[2]], writes=[b_rt])
                R = [b_rt]
                S.op(dve, lambda: nc.vector.tensor_tensor(out=sel[:], in0=sc[:], in1=rb_bc[:, 0:256], op=ALU.add),
                     reads=R + [b_const], writes=R)
                for g in range(8):
                    S.op(dve, lambda g=g: nc.vector.max(out=m8[:, g, :], in_=sel[:, g * 32:(g + 1) * 32]), reads=R,
                         writes=R)
                S.op(dve, lambda: nc.vector.tensor_tensor(out=gsc[:], in0=m8[:, :, 0], in1=m8[:, :, 1], op=ALU.add),
                     reads=R, writes=R)
                S.op(dve, lambda: nc.vector.max(out=gm8[:], in_=gsc[:]), reads=R, writes=R)
                S.op(dve, lambda: nc.vector.tensor_scalar(out=gmask[:], in0=gsc[:], scalar1=gm8[:, 3:4], scalar2=None,
                                                          op0=ALU.is_ge), reads=R, writes=R)
                S.op(dve, lambda: nc.vector.tensor_scalar(out=pen[:], in0=gmask[:], scalar1=10.0, scalar2=-10.0,
                                                          op0=ALU.mult, op1=ALU.add), reads=R, writes=R)
                for g in range(8):
                    S.op(dve, lambda g=g: nc.vector.tensor_scalar(out=selm[:, g * 32:(g + 1) * 32],
                                                                  in0=sel[:, g * 32:(g + 1) * 32],
                                                                  scalar1=gmask[:, g:g + 1], scalar2=pen[:, g:g + 1],
                                                                  op0=ALU.mult, op1=ALU.add), reads=R, writes=R)
                S.op(dve, lambda: nc.vector.max(out=t8[:], in_=selm[:]), reads=R, writes=R)
                S.op(dve, lambda: nc.vector.tensor_scalar(out=selm[:], in0=selm[:], scalar1=t8[:, 7:8], scalar2=None,
                                                          op0=ALU.is_ge), reads=R, writes=R)
                S.op(act, lambda t=t: nc.scalar.copy(out=Mb[:, t, :], in_=selm[:]), reads=R, writes=[b_Mb])
                S.op(dve, lambda: nc.vector.tensor_tensor(out=sel[:], in0=selm[:], in1=sc[:], op=ALU.mult), reads=R,
                     writes=R)
                S.op(dve, lambda: nc.vector.reduce_sum(out=den[:], in_=sel[:], axis=AX.X), reads=R, writes=R)
                S.op(dve, lambda: nc.vector.reciprocal(out=den[:], in_=den[:]), reads=R, writes=R)
                S.op(dve, lambda t=t: nc.vector.tensor_scalar(out=Gt[:, t, 0:NE], in0=sel[:], scalar1=den[:, 0:1],
                                                              scalar2=2.5, op0=ALU.mult, op1=ALU.mult),
                     reads=R + [b_G], writes=R + [b_G])
            for t in range(8):
                for t2 in range(t):
                    S.op(pe, lambda t2=t2: nc.tensor.matmul(banks[3][:, 0:NE], ones_bf[:], Mb[:, t2, :], start=(t2 == 0),
                                                            stop=False), reads=[b_Mb, b_const], writes=[pb[3]],
                         signal=False)
                S.op(pe, lambda t=t: nc.tensor.matmul(banks[3][:, 0:NE], triS_bf[:], Mb[:, t, :], start=(t == 0),
                                                      stop=True), reads=[b_Mb, b_const], writes=[pb[3]])
                S.op(dve, lambda t=t: nc.vector.scalar_tensor_tensor(out=rankp[:, t, :], in0=banks[3][:, 0:NE], scalar=1.0,
                                                                     in1=Mb[:, t, :], op0=ALU.add, op1=ALU.mult),
                     reads=[pb[3], b_Mb], writes=[b_rank])
                S.op(dve, lambda t=t: nc.vector.tensor_scalar_add(out=rankp[:, t, :], in0=rankp[:, t, :], scalar1=-1.0),
                     reads=[b_rank], writes=[b_rank])
            rowA = sb("rowA", [1, NE], F32, phB)
            rowB = sb("rowB", [1, NE], F32, phB)
            nbr = sb("nbr", [1, NE], F32, phB)
            pcol = sb("pcol", [128, 2], F32, phB)
            iob = sb("iob", [128, 384], F32, phB)
            cmpb = sb("cmpb", [128, 2, 384], BF16, phB)
            psb = sb("psb", [128, NE], F32, phB)
            b_row = Buf("rows")
            S.dma(sp, iob[:], tri_d[:, 640:1024], writes=[b_row])
            for t in range(8):
                S.op(pe, lambda t=t: nc.tensor.matmul(banks[3][0:1, 0:NE], ones_bf[:, 0:1], Mb[:, t, :], start=(t == 0),
                                                      stop=(t == 7)), reads=[b_Mb, b_const], writes=[pb[3]],
                     signal=(t == 7))
            RW = [b_row]
            S.op(dve, lambda: nc.vector.tensor_copy(out=rowB[:], in_=banks[3][0:1, 0:NE]), reads=[pb[3]] + RW, writes=RW)
            S.op(dve, lambda: nc.vector.tensor_scalar(out=nbr[:], in0=rowB[:], scalar1=0.0, scalar2=None, op0=ALU.is_gt),
                 reads=RW, writes=RW)
            for j_ in range(1, 8):
                S.op(dve, lambda j_=j_: nc.vector.scalar_tensor_tensor(out=nbr[:], in0=rowB[:], scalar=128.0 * j_,
                                                                       in1=nbr[:], op0=ALU.is_gt, op1=ALU.add),
                     reads=RW, writes=RW)
            S.op(dve, lambda: nc.vector.tensor_copy(out=rowA[:], in_=nbr[:]), reads=RW, writes=RW)
            cur, nxt = rowA, rowB
            sft = 1
            while sft < NE:
                S.op(dve, lambda cur=cur, nxt=nxt, sft=sft: nc.vector.tensor_copy(out=nxt[:, 0:sft], in_=cur[:, 0:sft]),
                     reads=RW, writes=RW)
                S.op(dve, lambda cur=cur, nxt=nxt, sft=sft: nc.vector.tensor_add(out=nxt[:, sft:NE], in0=cur[:, sft:NE],
                                                                                in1=cur[:, 0:NE - sft]), reads=RW,
                     writes=RW)
                cur, nxt = nxt, cur
                sft *= 2
            pend = cur
            for hf_ in range(2):
                S.op(pe, lambda hf_=hf_: nc.tensor.matmul(banks[3][:, 300 + hf_:301 + hf_],
                                                          pend[0:1, hf_ * 128:(hf_ + 1) * 128], ones_f[0:1, 0:1],
                                                          start=True, stop=True), reads=RW + [b_const], writes=[pb[3]],
                     signal=(hf_ == 1))
            S.op(dve, lambda: nc.vector.tensor_copy(out=pcol[:], in_=banks[3][:, 300:302]), reads=[pb[3]], writes=RW)
            S.op(dve, lambda: nc.vector.tensor_sub(out=nxt[:], in0=pend[:], in1=nbr[:]), reads=RW, writes=RW)
            pst = nxt
            S.op(pe, lambda: nc.tensor.matmul(banks[2][:, 0:NE], ones_f[0:1, :], pst[0:1, :], start=True, stop=True),
                 reads=RW + [b_const], writes=[pb[2]])
            S.op(dve, lambda: nc.vector.tensor_copy(out=psb[:], in_=banks[2][:, 0:NE]), reads=[pb[2]], writes=RW)
            for t in range(8):
                S.op(dve, lambda t=t: nc.vector.scalar_tensor_tensor(out=sel[:], in0=psb[:], scalar=128.0, in1=Mb[:, t, :],
                                                                     op0=ALU.mult, op1=ALU.mult),
                     reads=RW + [b_Mb, b_rt], writes=[b_rt])
                S.op(dve, lambda t=t: nc.vector.tensor_add(out=rankp[:, t, :], in0=rankp[:, t, :], in1=sel[:]),
                     reads=[b_rt, b_rank], writes=[b_rank])
            for hf_ in range(2):
                S.op(dve, lambda hf_=hf_: nc.vector.tensor_scalar(out=cmpb[:, hf_, :], in0=iob[:],
                                                                  scalar1=pcol[:, hf_:hf_ + 1], scalar2=None,
                                                                  op0=ALU.is_ge), reads=RW, writes=RW)
            for hf_ in range(2):
                S.op(pe, lambda hf_=hf_: nc.tensor.matmul(banks[3][:, 0:384], ones_bf[:], cmpb[:, hf_, :],
                                                          start=(hf_ == 0), stop=(hf_ == 1)), reads=RW + [b_const],
                     writes=[pb[3]], signal=(hf_ == 1))
            p4 = sb("p4", [128, 2], F32, phB)
            S.dma(sp, p4[:], tri_d[:, 1024:1026], writes=RW)
            S.op(dve, lambda: nc.vector.tensor_scalar_min(out=iob[:], in0=banks[3][:, 0:384],
                                                          scalar1=float(n_exp - 1)), reads=[pb[3]] + RW, writes=RW)
            S.op(dve, lambda: nc.vector.tensor_scalar(out=iob[:], in0=iob[:], scalar1=512.0, scalar2=None,
                                                      op0=ALU.mult), reads=RW, writes=RW)
            S.op(dve, lambda: nc.vector.tensor_scalar(out=iob[:], in0=iob[:], scalar1=p4[:, 0:1], scalar2=None,
                                                      op0=ALU.add), reads=RW, writes=RW)
            S.op(dve, lambda: nc.vector.tensor_copy(out=eb_i[:], in_=iob[:]), reads=RW, writes=[b_eb])
            for t in range(8):
                S.op(dve, lambda t=t: nc.vector.max(out=gate8[:, t * 8:(t + 1) * 8], in_=Gt[:, t, 0:NE]), reads=[b_G],
                     writes=[b_c8])
                for k in range(8):
                    S.op(dve, lambda t=t, k=k: nc.vector.tensor_scalar(out=sel[:], in0=Gt[:, t, 0:NE],
                                                                       scalar1=gate8[:, t * 8 + k:t * 8 + k + 1],
                                                                       scalar2=None, op0=ALU.is_equal),
                         reads=[b_G, b_c8, b_rt], writes=[b_rt])
                    S.op(dve, lambda t=t: nc.vector.tensor_tensor(out=sel[:], in0=sel[:], in1=rankp[:, t, :], op=ALU.mult),
                         reads=[b_rt, b_rank], writes=[b_rt])
                    S.op(dve, lambda t=t, k=k: nc.vector.reduce_sum(out=code8[:, t * 8 + k:t * 8 + k + 1], in_=sel[:],
                                                                    axis=AX.X), reads=[b_rt], writes=[b_c8])
            b_dbg = Buf("dbg")
            if debug:
                S.dma(sp, dbg_g, Gt[:], reads=[b_G], writes=[b_dbg])
                S.dma(sp, dbg_rank, rankp[:], reads=[b_rank], writes=[b_dbg])
                S.dma(sp, dbg_c8[:, 0:64], code8[:], reads=[b_c8], writes=[b_dbg])
                S.dma(sp, dbg_c8[:, 64:128], gate8[:], reads=[b_c8], writes=[b_dbg])
                S.dma(sp, dbg_eb, eb_i[:], reads=[b_eb], writes=[b_dbg])
            all_wait([b_wo, b_bc, b_wr, b_h2f, b_pre, b_st6, b_mv, b_rt, b_xt, b_dbg, b_Mb, b_row, b_c8, b_eb] + b_x1d + b_mix)

        with ExitStack() as phC:
            yacc = sb("yacc", [128, 8, D], F32, phC)
            b_yn = [[Buf(f"y{t}_{n}") for n in range(4)] for t in range(8)]
            b_y = [None] * 8
            S.op(dve, lambda: nc.vector.memset(yacc[:], 0.0), writes=[b for row in b_yn for b in row])
            with ExitStack() as phW:
                wg = sb("wg", [128, KC, FF], BF16, phW)
                wu = sb("wu", [128, KC, FF], BF16, phW)
                wd = sb("wd", [128, 4, D], BF16, phW)
                b_wg = [Buf(f"wg{i}") for i in range(4)]
                b_wu = [Buf(f"wu{i}") for i in range(4)]
                b_wd = [Buf(f"wd{i}") for i in range(4)]
                Se = [sb(f"Se{i}", [128, 8, 128], BF16, phW) for i in range(2)]
                b_Se = [[Buf(f"Se{i}_{t}") for t in range(8)] for i in range(2)]
                SeT = sb("SeT", [128, 8, 128], BF16, phW)
                b_SeT = Buf("SeT")
                xeT = sb("xeT", [128, KC, 128], BF16, phW)
                b_xeT = Buf("xeT")
                sgt = sb("sgt", [128, FF], BF16, phW)
                b_sgt = Buf("sgt")
                actm = sb("actm", [128, FF], BF16, phW)
                b_actm = Buf("actm")
                actT = sb("actT", [128, 4, 128], BF16, phW)
                b_actT = Buf("actT")
                ye = sb("ye", [128, D], BF16, phW)
                b_ye = Buf("ye")

                wg_rows = wg_all.rearrange("e p (q k) c -> (e p q) (k c)", q=4)
                wu_rows = wu_all.rearrange("e p (q k) c -> (e p q) (k c)", q=4)
                wd_rows = wd_all.rearrange("e p j c -> (e p j) c")

                def load_weights(slot):
                    for i in range(4):
                        S.dma(pool, wg[:, 4 * i:4 * i + 4, :], wg_all[slot, :, 4 * i:4 * i + 4, :], writes=[b_wg[i]])
                    for i in range(4):
                        S.dma(pool, wu[:, 4 * i:4 * i + 4, :], wu_all[slot, :, 4 * i:4 * i + 4, :], writes=[b_wu[i]])
                    for i in range(4):
                        S.dma(pool, wd[:, i:i + 1, :], wd_all[slot, :, i:i + 1, :], writes=[b_wd[i]])

                def load_weights_block(b):
                    ix = eb_i[:, b:b + 1]
                    for i in range(4):
                        S.dma_gather(wg[:, 4 * i:4 * i + 4, :].rearrange("p k c -> p (k c)"), wg_rows, ix, i * 2048,
                                     reads=[b_eb], writes=[b_wg[i]])
                    for i in range(4):
                        S.dma_gather(wu[:, 4 * i:4 * i + 4, :].rearrange("p k c -> p (k c)"), wu_rows, ix, i * 2048,
                                     reads=[b_eb], writes=[b_wu[i]])
                    for i in range(4):
                        S.dma_gather(wd[:, i, :], wd_rows, ix, i * 2048, reads=[b_eb], writes=[b_wd[i]])

                key8 = sb("key8", [128, 64], F32, phW)
                g1t = sb("g1t", [128, 64], F32, phW)
                gsel = [sb(f"gsel{i}", [128, 8], F32, phW) for i in range(2)]
                b_key, b_g1t = Buf("key8"), Buf("g1t")
                b_gsel = [Buf("gsel0"), Buf("gsel1")]

                def expert_body(gather, combine, gate_fn, b_gate, mid_hook=None):
                    ng = len(gather)
                    for q in range(4):
                        bk = q % 2
                        for kk in range(4):
                            k = q * 4 + kk
                            for gi, (t, s_ap, b_s) in enumerate(gather):
                                S.op(pe, lambda k=k, kk=kk, t=t, s_ap=s_ap, gi=gi, bk=bk: nc.tensor.matmul(
                                    banks[bk][:, kk * 128:(kk + 1) * 128], h2tm[:, t, k * 128:(k + 1) * 128], s_ap,
                                    start=(gi == 0), stop=(gi == ng - 1)), reads=[b_h2[t], b_s], writes=[pb[bk]],
                                    signal=(kk == 3 and gi == ng - 1))
                        S.op(act, lambda q=q, bk=bk: nc.scalar.copy(
                            out=xeT[:, 4 * q:4 * q + 4, :], in_=banks[bk][:, :].rearrange("p (a b) -> p a b", a=4)),
                            reads=[pb[bk]], writes=[b_xeT])
                    for k in range(KC):
                        S.op(pe, lambda k=k: nc.tensor.matmul(banks[2][:, :], xeT[:, k, :], wg[:, k, :], start=(k == 0),
                                                              stop=(k == KC - 1)), reads=[b_xeT, b_wg[k // 4]],
                             writes=[pb[2]], signal=(k == KC - 1))
                    for k in range(KC):
                        S.op(pe, lambda k=k: nc.tensor.matmul(banks[3][:, :], xeT[:, k, :], wu[:, k, :], start=(k == 0),
                                                              stop=(k == KC - 1)), reads=[b_xeT, b_wu[k // 4]],
                             writes=[pb[3]], signal=(k == KC - 1))
                    if mid_hook is not None:
                        mid_hook()
                    S.op(act, lambda: nc.scalar.activation(out=sgt[:], in_=banks[2][:, :], func=AF.Silu), reads=[pb[2]],
                         writes=[b_sgt])
                    S.op(dve, lambda: nc.vector.tensor_tensor(out=actm[:], in0=banks[3][:, :], in1=sgt[:], op=ALU.mult),
                         reads=[pb[3], b_sgt], writes=[b_actm])
                    for j in range(4):
                        S.op(pe, lambda j=j: nc.tensor.matmul(banks[0][:, j * 128:(j + 1) * 128],
                                                              actm[:, j * 128:(j + 1) * 128], ident_bf[:], start=True,
                                                              stop=True), reads=[b_actm, b_const], writes=[pb[0]],
                             signal=(j == 3))
                    S.op(act, lambda: nc.scalar.copy(out=actT[:], in_=banks[0][:, :].rearrange("p (a b) -> p a b", a=4)),
                         reads=[pb[0]], writes=[b_actT])
                    for j in range(4):
                        for n in range(4):
                            S.op(pe, lambda j=j, n=n: nc.tensor.matmul(banks[4 + n][:, :], actT[:, j, :],
                                                                       wd[:, j, n * 512:(n + 1) * 512], start=(j == 0),
                                                                       stop=(j == 3)), reads=[b_actT, b_wd[j]],
                                 writes=[pb[4 + n]], signal=(j == 3))
                    for n in range(4):
                        if n % 2 == 0:
                            S.op(act, lambda n=n: nc.scalar.copy(out=ye[:, n * 512:(n + 1) * 512], in_=banks[4 + n][:, :]),
                                 reads=[pb[4 + n]], writes=[b_ye])
                        else:
                            S.op(dve, lambda n=n: nc.vector.tensor_copy(out=ye[:, n * 512:(n + 1) * 512],
                                                                        in_=banks[4 + n][:, :]), reads=[pb[4 + n]],
                                 writes=[b_ye])
                    ci = 0
                    for (t, st_ap, b_st) in combine:
                        for n in range(4):
                            bk = ci % 2
                            ci += 1
                            S.op(pe, lambda st_ap=st_ap, n=n, bk=bk: nc.tensor.matmul(
                                banks[bk][:, :], st_ap, ye[:, n * 512:(n + 1) * 512], start=True, stop=True),
                                reads=[b_st, b_ye], writes=[pb[bk]])
                            S.op(dve, lambda t=t, n=n, bk=bk: nc.vector.scalar_tensor_tensor(
                                out=yacc[:, t, n * 512:(n + 1) * 512], in0=banks[bk][:, :],
                                scalar=gate_fn(t), in1=yacc[:, t, n * 512:(n + 1) * 512], op0=ALU.mult,
                                op1=ALU.add), reads=[pb[bk], b_gate, b_yn[t][n]], writes=[b_yn[t][n]])

                def prepare(b):
                    p = b % 2
                    S.op(dve, lambda b=b: nc.vector.tensor_scalar_add(out=key8[:], in0=code8[:], scalar1=-128.0 * b),
                         reads=[b_c8], writes=[b_key])
                    for k in range(8):
                        for t in range(8):
                            c = t * 8 + k
                            if k == 0:
                                S.op(dve, lambda t=t, c=c, p=p: nc.vector.tensor_scalar(
                                    out=Se[p][:, t, :], in0=iota_f[:], scalar1=key8[:, c:c + 1], scalar2=None,
                                    op0=ALU.is_equal), reads=[b_key, b_const], writes=[b_Se[p][t]])
                            else:
                                S.op(dve, lambda t=t, c=c, p=p: nc.vector.scalar_tensor_tensor(
                                    out=Se[p][:, t, :], in0=iota_f[:], scalar=key8[:, c:c + 1], in1=Se[p][:, t, :],
                                    op0=ALU.is_equal, op1=ALU.add), reads=[b_key, b_const, b_Se[p][t]],
                                    writes=[b_Se[p][t]])
                    S.op(dve, lambda: nc.vector.tensor_scalar(out=g1t[:], in0=key8[:], scalar1=0.0, scalar2=None,
                                                              op0=ALU.is_ge), reads=[b_key], writes=[b_g1t])
                    S.op(dve, lambda: nc.vector.scalar_tensor_tensor(out=g1t[:], in0=key8[:], scalar=128.0, in1=g1t[:],
                                                                     op0=ALU.is_lt, op1=ALU.mult), reads=[b_key, b_g1t],
                         writes=[b_g1t])
                    S.op(dve, lambda: nc.vector.tensor_tensor(out=g1t[:], in0=g1t[:], in1=gate8[:], op=ALU.mult),
                         reads=[b_g1t, b_c8], writes=[b_g1t])
                    S.op(dve, lambda p=p: nc.vector.reduce_sum(out=gsel[p][:], in_=g1t[:].rearrange("p (t k) -> p t k", k=8),
                                                               axis=AX.X), reads=[b_g1t], writes=[b_gsel[p]])

                prepare(0)
                for b in range(nblk):
                    p = b % 2
                    load_weights_block(b)
                    for half in range(2):
                        for tt in range(4):
                            t = half * 4 + tt
                            S.op(pe, lambda t=t, tt=tt, p=p: nc.tensor.matmul(
                                banks[2 + half][:, tt * 128:(tt + 1) * 128], Se[p][:, t, :], ident_bf[:], start=True,
                                stop=True), reads=[b_Se[p][t], b_const], writes=[pb[2 + half]], signal=(tt == 3))
                        S.op(act, lambda half=half: nc.scalar.copy(
                            out=SeT[:, 4 * half:4 * half + 4, :],
                            in_=banks[2 + half][:, :].rearrange("p (a b) -> p a b", a=4)), reads=[pb[2 + half]],
                            writes=[b_SeT])
                    expert_body([(t, Se[p][:, t, :], b_Se[p][t]) for t in range(8)],
                                [(t, SeT[:, t, :], b_SeT) for t in range(8)],
                                lambda t, p=p: gsel[p][:, t:t + 1], b_gsel[p],
                                mid_hook=(lambda b=b: prepare(b + 1)) if b + 1 < nblk else None)
                load_weights(n_exp - 1)
                for t in range(8):
                    expert_body([(t, ident_bf[:], b_const)], [(t, ident_bf[:], b_const)],
                                lambda t: Gt[:, t, NE:NE + 1], b_G)
                all_wait(b_wg + b_wu + b_wd + b_Se[0] + b_Se[1] + b_gsel + [b_SeT, b_xeT, b_sgt, b_actm, b_actT, b_ye, b_key, b_g1t])

            x1t = sb("x1t", [128, D], F32, phC)
            b_x1t = Buf("x1t")
            l2 = sb("l2", [128, 2 * D], F32, phC)
            b_l2 = Buf("l2")
            st6 = sb("st6d", [128, 24], F32, phC)
            b_st6 = Buf("st6d")
            mv = sb("mvd", [128, 4], F32, phC)
            b_mv = Buf("mvd")
            S.dma(sp, l2[:], bcv[:, 4 * D:6 * D], writes=[b_l2])
            outs = []
            for t in range(8):
                b_y[t] = Buf(f"y{t}")
                S.dma(sp, x1t[:], x1_d[t * 128:(t + 1) * 128, :], reads=[b_x1d[t]], writes=[b_x1t])
                S.op(dve, lambda t=t: nc.vector.tensor_tensor(out=yacc[:, t, :], in0=yacc[:, t, :], in1=g_bc[:, D:2 * D],
                                                              op=ALU.mult), reads=b_yn[t] + [b_mod],
                     writes=[b_y[t]] + b_yn[t])
                S.op(dve, lambda t=t: nc.vector.scalar_tensor_tensor(out=yacc[:, t, :], in0=x1t[:], scalar=ALPHA,
                                                                     in1=yacc[:, t, :], op0=ALU.mult, op1=ALU.add),
                     reads=[b_x1t, b_y[t]], writes=[b_y[t]])
                layer_norm_stats(yacc[:, t, :], b_y[t], mv[:, 0:2], b_mv, st6, b_st6)
                ln_normalize(yacc[:, t, :], b_y[t], mv[:, 0:4], b_mv)
                S.op(dve, lambda t=t: nc.vector.tensor_tensor(out=yacc[:, t, :], in0=yacc[:, t, :], in1=l2[:, 0:D],
                                                              op=ALU.mult), reads=[b_y[t], b_l2], writes=[b_y[t]])
                S.op(dve, lambda t=t: nc.vector.tensor_tensor(out=yacc[:, t, :], in0=yacc[:, t, :], in1=l2[:, D:2 * D],
                                                              op=ALU.add), reads=[b_y[t], b_l2], writes=[b_y[t]])
                b_o = Buf(f"o{t}")
                S.dma(sp, out_d[t * 128:(t + 1) * 128, :], yacc[:, t, :], reads=[b_y[t]], writes=[b_o])
                outs.append(b_o)
            all_wait(outs + b_y + [b_x1t, b_l2, b_G, b_st6, b_mv, b_mod, b_const, b_stats] + b_x1d + b_h2 + [b_rank] + pb)
    return nc


def pool_or_dve(S):
    return S.dve


def _masks():
    k = np.arange(128)[:, None]
    q = np.arange(128)[None, :]
    d = q - k
    m4 = (d % 4 == 0).astype(np.float32)
    m16 = (d % 16 == 0).astype(np.float32)
    le = (k <= q).astype(np.float32)
    ge = (k >= q).astype(np.float32)
    out = np.zeros((128, NMASK, 128), np.float32)
    for delta in range(-3, 16):
        if delta < 0:
            m = np.zeros((128, 128), np.float32)
        elif delta == 0:
            m = le * (1.0 + m4 + m16)
        elif delta == 1:
            m = ge + m4 + m16
        elif delta in (2, 3):
            m = m4 + m16
        elif delta == 4:
            m = ge * m4 + m16
        else:
            m = m16.copy()
        out[:, delta + 3, :] = m
    return out.reshape(128, NMASK * 128)


def _tri():
    j = np.arange(128)[:, None]
    i = np.arange(128)[None, :]
    ident = np.eye(128, dtype=np.float32)
    tu = (j <= i).astype(np.float32) / 16.0
    tl = (j > i).astype(np.float32) / 16.0
    ts = (j < i).astype(np.float32)
    io = np.broadcast_to(np.arange(128, dtype=np.float32)[None, :], (128, 128))
    io3 = np.broadcast_to(np.arange(384, dtype=np.float32)[None, :], (128, 384))
    p4 = np.broadcast_to((4.0 * np.arange(128, dtype=np.float32))[:, None], (128, 128))
    return np.ascontiguousarray(np.concatenate([ident, tu, tl, ts, io, io3, p4], axis=1))


def _fm(v):
    return np.ascontiguousarray(v.reshape(KC, 128).T)


def _ktile(w):
    return np.ascontiguousarray(w.reshape(KC, 128, w.shape[1]).transpose(1, 0, 2))


def prepare_inputs(x, c, ln_in_g, ln_in_b, w_ada, b_ada, w_in, w_gla_gate, b_gla_gate, gla_norm_w, w_out,
                   ln1_g, ln1_b, w_router, router_bias, w_sh_gate, w_sh_up, w_sh_down,
                   w_exp_gate, w_exp_up, w_exp_down, ln2_g, ln2_b, cores=range(8), n_exp=NE + 1):
    f = np.float32
    x = np.asarray(x, f)
    shared = {}
    shared["fmv"] = np.ascontiguousarray(np.concatenate([_fm(np.asarray(ln_in_g, f)), _fm(np.asarray(ln_in_b, f)),
                                                         _fm(np.asarray(ln1_g[0], f)), _fm(np.asarray(ln1_b[0], f))], 1))
    rows = np.concatenate([np.asarray(ln_in_g, f), np.asarray(ln_in_b, f), np.asarray(ln1_g[0], f),
                           np.asarray(ln1_b[0], f), np.asarray(ln2_g[0], f), np.asarray(ln2_b[0], f),
                           np.asarray(router_bias[0], f), np.asarray(gla_norm_w[0], f)])
    shared["bcv"] = np.ascontiguousarray(np.broadcast_to(rows[None, :], (128, rows.size)))
    wa = np.asarray(w_ada[0], f).reshape(KC, 128, 24, 512).transpose(2, 1, 0, 3)
    shared["w_ada"] = np.ascontiguousarray(wa)
    shared["b_ada"] = np.ascontiguousarray(np.asarray(b_ada, f).reshape(1, 6 * D))
    wi = np.zeros((D, 49 * 128), f)
    wi[:, :IN_W] = np.asarray(w_in[0], f)
    shared["w_in"] = np.ascontiguousarray(wi.reshape(KC, 128, 49, 128).transpose(2, 1, 0, 3))
    shared["wgate"] = np.ascontiguousarray(np.concatenate([np.asarray(w_gla_gate[0], f),
                                                           np.asarray(b_gla_gate, f).reshape(1, 512)], 0))
    shared["w_out"] = _ktile(np.asarray(w_out[0], f))
    shared["w_router"] = _ktile(np.asarray(w_router[0], f))
    nr = n_exp - 1
    wg = np.empty((n_exp, 128, KC, FF), f)
    wu = np.empty((n_exp, 128, KC, FF), f)
    wd = np.empty((n_exp, 128, 4, D), f)
    wg[:nr] = np.asarray(w_exp_gate[0][:nr], f).reshape(nr, KC, 128, FF).transpose(0, 2, 1, 3)
    wu[:nr] = np.asarray(w_exp_up[0][:nr], f).reshape(nr, KC, 128, FF).transpose(0, 2, 1, 3)
    wd[:nr] = np.asarray(w_exp_down[0][:nr], f).reshape(nr, 4, 128, D).transpose(0, 2, 1, 3)
    wg[nr] = _ktile(np.asarray(w_sh_gate[0], f))
    wu[nr] = _ktile(np.asarray(w_sh_up[0], f))
    wd[nr] = np.asarray(w_sh_down[0], f).reshape(4, 128, D).transpose(1, 0, 2)
    shared["wg_all"], shared["wu_all"], shared["wd_all"] = wg, wu, wd
    shared["masks"] = _masks()
    shared["tri"] = _tri()
    maps = []
    for core in cores:
        b, hf = core // 2, core % 2
        m = dict(shared)
        if hf == 1:
            m["xc"] = np.ascontiguousarray(x[b])
        else:
            m["xc"] = np.ascontiguousarray(np.concatenate([x[b, :OWN], x[b, :OWN]], 0))
        m["flag"] = np.full((128, 1), float(hf), f)
        m["cfm"] = _fm(np.asarray(c[b], f))
        maps.append(m)
    return maps


def kernel(**inputs):
    maps = prepare_inputs(**inputs)
    nc = build_program()
    res = run_bass_kernel_spmd(nc, maps, core_ids=list(range(8)))
    out = np.empty((4, SEQ, D), np.float32)
    for core in range(8):
        b, hf = core // 2, core % 2
        out[b, hf * OWN:(hf + 1) * OWN] = res.results[core]["out"]
    return out
```
